# Optimizing a Trainium2 kernel written in Bass

```python
import jax
import jax.numpy as jnp
from jax import lax
import numpy as np

D_MODEL = 2048
BATCH = 1
SEQ = 8192
DEPTH = 4

GRID_W = 64
NA_HEADS = 16
NA_HEAD_DIM = 64
NA_WIDTH = NA_HEADS * NA_HEAD_DIM
WIN_ROWS = 8
WIN_COLS = 16
F_GROUPS = 4
F_GROUP_DIM = 256
F_WIDTH = F_GROUPS * F_GROUP_DIM
MIX_IN_WIDTH = 3 * NA_WIDTH + F_WIDTH
N_BRANCHES = 2
N_GROUPS = 4
EXPERTS_PER_GROUP = 8
N_EXPERTS = N_GROUPS * EXPERTS_PER_GROUP
TOP_K = 2
D_EXPERT = 512
EXPERT_BLOCK = 128
N_MOD = 6
EPS = 1e-6
NEG_INF = -1e30

kernel_name = 'hybrid_na_fnet_hmoe_encoder'


def rmsnorm(x, g):
    xf = x.astype(jnp.float32)
    y = xf * lax.rsqrt(jnp.mean(xf * xf, axis=-1, keepdims=True) + EPS)
    return (y * g.astype(jnp.float32)).astype(x.dtype)


def modulate(h, shift, scale):
    return h * (1 + scale[:, None, :]) + shift[:, None, :]


def neighborhood_attention(q, k, v, rpb):
    b, s, nh, dh = q.shape
    rows = s // GRID_W
    kh = min(WIN_ROWS, rows)
    r = jnp.arange(rows)
    row_start = jnp.clip(r - kh // 2, 0, rows - kh)
    key_rows = row_start[:, None] + jnp.arange(kh)[None, :]
    col = jnp.arange(GRID_W)
    col_start = jnp.clip(col - WIN_COLS // 2, 0, GRID_W - WIN_COLS)
    col_mask = (col[None, :] >= col_start[:, None]) & (col[None, :] < col_start[:, None] + WIN_COLS)
    qg = q.reshape(b, rows, GRID_W, nh, dh) * (dh ** -0.5)
    kr = k.reshape(b, rows, GRID_W, nh, dh)[:, key_rows]
    vr = v.reshape(b, rows, GRID_W, nh, dh)[:, key_rows]
    scores = jnp.einsum('brchd,brkjhd->bhrckj', qg, kr).astype(jnp.float32)
    dr = key_rows - r[:, None] + (WIN_ROWS - 1)
    dc = jnp.clip(col[None, :] - col[:, None], -(WIN_COLS - 1), WIN_COLS - 1) + (WIN_COLS - 1)
    bias = jnp.take(rpb[:, dr], dc, axis=3)
    bias = jnp.transpose(bias, (0, 1, 3, 2, 4)).astype(jnp.float32)
    scores = jnp.where(col_mask[:, None, :], scores + bias[None], NEG_INF)
    probs = jax.nn.softmax(scores, axis=(-2, -1)).astype(v.dtype)
    out = jnp.einsum('bhrckj,brkjhd->brchd', probs, vr)
    return out.reshape(b, s, nh * dh)


def fourier_mix(u):
    b, s, _ = u.shape
    ug = u.reshape(b, s, F_GROUPS, F_GROUP_DIM).astype(jnp.float32)
    y = jnp.fft.fft2(ug, axes=(1, 3), norm='ortho').real
    return y.reshape(b, s, F_WIDTH).astype(u.dtype)


def mixer(h, w_in, rpb, w_na_o, w_f_o, w_bg, b_bg, w_out):
    b, s, _ = h.shape
    proj = h @ w_in
    q, k, v, u = jnp.split(proj, [NA_WIDTH, 2 * NA_WIDTH, 3 * NA_WIDTH], axis=-1)
    q = q.reshape(b, s, NA_HEADS, NA_HEAD_DIM)
    k = k.reshape(b, s, NA_HEADS, NA_HEAD_DIM)
    v = v.reshape(b, s, NA_HEADS, NA_HEAD_DIM)
    y_na = neighborhood_attention(q, k, v, rpb) @ w_na_o
    y_f = fourier_mix(u) @ w_f_o
    gates = jax.nn.sigmoid(h @ w_bg + b_bg)
    g_na, g_f = jnp.split(gates, N_BRANCHES, axis=-1)
    return (g_na * y_na + g_f * y_f) @ w_out


def grouped_expert_ffn(hf, e_flat, w_flat, w_gate, w_up, w_down):
    t, d = hf.shape
    a = e_flat.shape[0]
    tok = jnp.repeat(jnp.arange(t, dtype=jnp.int32), TOP_K)
    order = jnp.argsort(e_flat)
    se = e_flat[order]
    stok = tok[order]
    sw = w_flat[order]
    counts = jnp.bincount(e_flat, length=N_EXPERTS)
    starts = jnp.cumsum(counts) - counts
    pcounts = (counts + EXPERT_BLOCK - 1) // EXPERT_BLOCK * EXPERT_BLOCK
    pends = jnp.cumsum(pcounts)
    pstarts = pends - pcounts
    rank = jnp.arange(a) - starts[se]
    dest = pstarts[se] + rank
    p = a + N_EXPERTS * EXPERT_BLOCK
    nb = p // EXPERT_BLOCK
    buf_tok = jnp.full((p,), t, dtype=jnp.int32).at[dest].set(stok)
    xpad = jnp.concatenate([hf, jnp.zeros((1, d), hf.dtype)], axis=0)
    xb = xpad[buf_tok].reshape(nb, EXPERT_BLOCK, d)
    block_e = jnp.minimum(jnp.searchsorted(pends, jnp.arange(nb) * EXPERT_BLOCK, side='right'), N_EXPERTS - 1)

    def expert_block(args):
        xblk, e = args
        gate = xblk @ w_gate[e]
        up = xblk @ w_up[e]
        return (jax.nn.silu(gate) * up) @ w_down[e]

    yb = lax.map(expert_block, (xb, block_e)).reshape(p, d)
    contrib = yb[dest] * sw[:, None].astype(yb.dtype)
    return jax.ops.segment_sum(contrib, stok, num_segments=t)


def hier_moe(h, wg_r, bg_r, we_r, be_r, w_gate, w_up, w_down):
    b, s, d = h.shape
    t = b * s
    hf = h.reshape(t, d)
    g_probs = jax.nn.softmax((hf @ wg_r + bg_r).astype(jnp.float32), axis=-1)
    g_top_p, g_top = lax.top_k(g_probs, 1)
    e_logits = (hf @ we_r + be_r).astype(jnp.float32).reshape(t, N_GROUPS, EXPERTS_PER_GROUP)
    sel = e_logits[jnp.arange(t), g_top[:, 0]]
    l_top, l_idx = lax.top_k(sel, TOP_K)
    weights = g_top_p * jax.nn.softmax(l_top, axis=-1)
    experts = (g_top * EXPERTS_PER_GROUP + l_idx).astype(jnp.int32)
    y = grouped_expert_ffn(hf, experts.reshape(-1), weights.reshape(-1), w_gate, w_up, w_down)
    return y.reshape(b, s, d).astype(h.dtype)


def _normal(k, shape, scale):
    return jax.random.normal(k, shape, jnp.float32) * scale


def setup_inputs(seed: int = 0) -> dict:
    key = jax.random.key(seed)
    ks = jax.random.split(key, 24)
    L, D = DEPTH, D_MODEL
    return {
        'x': _normal(ks[0], (BATCH, SEQ, D), 1.0),
        'c': _normal(ks[1], (BATCH, D), 1.0),
        'norm1_g': 1.0 + _normal(ks[2], (L, D), 0.02),
        'norm2_g': 1.0 + _normal(ks[3], (L, D), 0.02),
        'ada_w': _normal(ks[4], (L, D, N_MOD * D), 0.5 * D ** -0.5),
        'ada_b': _normal(ks[5], (L, N_MOD * D), 0.02),
        'mix_in_w': _normal(ks[6], (L, D, MIX_IN_WIDTH), D ** -0.5),
        'na_rpb': _normal(ks[7], (L, NA_HEADS, 2 * WIN_ROWS - 1, 2 * WIN_COLS - 1), 0.1),
        'na_out_w': _normal(ks[8], (L, NA_WIDTH, D), NA_WIDTH ** -0.5),
        'fourier_out_w': _normal(ks[9], (L, F_WIDTH, D), F_WIDTH ** -0.5),
        'branch_gate_w': _normal(ks[10], (L, D, N_BRANCHES * D), D ** -0.5),
        'branch_gate_b': _normal(ks[11], (L, N_BRANCHES * D), 0.02),
        'mix_out_w': _normal(ks[12], (L, D, D), D ** -0.5),
        'router_group_w': _normal(ks[13], (L, D, N_GROUPS), D ** -0.5),
        'router_group_b': _normal(ks[14], (L, N_GROUPS), 0.01),
        'router_expert_w': _normal(ks[15], (L, D, N_EXPERTS), D ** -0.5),
        'router_expert_b': _normal(ks[16], (L, N_EXPERTS), 0.01),
        'expert_w_gate': _normal(ks[17], (L, N_EXPERTS, D, D_EXPERT), D ** -0.5),
        'expert_w_up': _normal(ks[18], (L, N_EXPERTS, D, D_EXPERT), D ** -0.5),
        'expert_w_down': _normal(ks[19], (L, N_EXPERTS, D_EXPERT, D), D_EXPERT ** -0.5),
        'final_g': 1.0 + _normal(ks[20], (D,), 0.02),
    }


def reference(x, c, norm1_g, norm2_g, ada_w, ada_b, mix_in_w, na_rpb, na_out_w, fourier_out_w,
              branch_gate_w, branch_gate_b, mix_out_w, router_group_w, router_group_b,
              router_expert_w, router_expert_b, expert_w_gate, expert_w_up, expert_w_down, final_g):
    cond = jax.nn.silu(c)
    for l in range(DEPTH):
        mod = cond @ ada_w[l] + ada_b[l]
        sh1, sc1, g1, sh2, sc2, g2 = jnp.split(mod, N_MOD, axis=-1)
        h = modulate(rmsnorm(x, norm1_g[l]), sh1, sc1)
        x = x + g1[:, None, :] * mixer(h, mix_in_w[l], na_rpb[l], na_out_w[l], fourier_out_w[l],
                                       branch_gate_w[l], branch_gate_b[l], mix_out_w[l])
        h = modulate(rmsnorm(x, norm2_g[l]), sh2, sc2)
        x = x + g2[:, None, :] * hier_moe(h, router_group_w[l], router_group_b[l], router_expert_w[l],
                                          router_expert_b[l], expert_w_gate[l], expert_w_up[l],
                                          expert_w_down[l])
    return rmsnorm(x, final_g)
```

```python
import numpy as np
import ml_dtypes
from contextlib import ExitStack
import concourse.bass as bass
import concourse.mybir as mybir
from concourse.bass_utils import run_bass_kernel_spmd

F32 = mybir.dt.float32
BF16 = mybir.dt.bfloat16
I32 = mybir.dt.int32
AF = mybir.ActivationFunctionType
ALU = mybir.AluOpType
AX = mybir.AxisListType

NCORE = 8
T = 1024
D = 2048
NL = 4
NE = 32
CAP = 384
DE = 512
ISQ_S = float(1.0 / np.sqrt(8192.0))
ENGS = ("pe", "act", "dve", "pool", "sp")
BF = ml_dtypes.bfloat16


class Buf:
    def __init__(self, name=""):
        self.name = name
        self.w = None
        self.r = []


class Sched:
    N_DMA_SEMS = 12

    def __init__(self, nc, es):
        self.nc = nc
        self._es = es
        self.q = {e: [] for e in ENGS}
        self.sem = {e: es.enter_context(nc.semaphore(f"s_{e}")) for e in ENGS}
        self.cnt = {e: 0 for e in ENGS}
        self.seen = {e: {} for e in ENGS}
        self.dsem, self.dcnt, self.dnext = {}, {}, {}
        self.semobj = {}
        for e in ("sp", "pool", "act"):
            self.dsem[e] = [es.enter_context(nc.semaphore(f"d_{e}{i}")) for i in range(self.N_DMA_SEMS)]
            self.dcnt[e] = [0] * self.N_DMA_SEMS
            self.dnext[e] = 0
            for i, s in enumerate(self.dsem[e]):
                self.semobj[("d", e, i)] = s
        for e in ENGS:
            self.semobj[("e", e)] = self.sem[e]
        self.ccsem = es.enter_context(nc.semaphore("s_cc"))
        self.cccnt = 0
        self.ccfg = 0
        self.semobj[("cc",)] = self.ccsem
        self.NBG = 8
        self.bgsem = [es.enter_context(nc.semaphore(f"bg{i}")) for i in range(self.NBG)]
        self.bgcnt = [0] * self.NBG
        self.bgnext = 0
        for i, s_ in enumerate(self.bgsem):
            self.semobj[("bg", i)] = s_

    def _wait(self, eng, tok):
        if tok is None:
            return
        key, val = tok
        if self.seen[eng].get(key, 0) >= val:
            return
        self.seen[eng][key] = val
        sem = self.semobj[key]
        self.q[eng].append(lambda E, sem=sem, val=val: E.wait_ge(sem, val))

    def _deps(self, eng, reads, writes, extra):
        toks = list(extra)
        for b in reads:
            toks.append(b.w)
        for b in writes:
            toks.append(b.w)
            toks.extend(b.r)
        for t in toks:
            self._wait(eng, t)

    @staticmethod
    def _commit(tok, reads, writes):
        for b in reads:
            b.r.append(tok)
        for b in writes:
            b.w = tok
            b.r = []

    def op(self, eng, fn, reads=(), writes=(), extra=()):
        self._deps(eng, reads, writes, extra)
        self.cnt[eng] += 1
        val = self.cnt[eng]
        sem = self.sem[eng]
        self.q[eng].append(lambda E, fn=fn, sem=sem: fn(E).then_inc(sem, 1))
        tok = (("e", eng), val)
        self._commit(tok, reads, writes)
        return tok

    def dma(self, eng, fn, reads=(), writes=(), extra=(), bg=False):
        if bg:
            i = self.bgnext
            self.bgnext = (i + 1) % self.NBG
            key = ("bg", i)
            if self.bgcnt[i] > 0:
                self._wait(eng, (key, self.bgcnt[i]))
            self._deps(eng, reads, writes, extra)
            self.bgcnt[i] += 16
            val = self.bgcnt[i]
            sem = self.bgsem[i]
            self.q[eng].append(lambda E, fn=fn, sem=sem: fn(E).then_inc(sem, 16))
            tok = (key, val)
            self._commit(tok, reads, writes)
            return tok
        i = self.dnext[eng]
        self.dnext[eng] = (i + 1) % self.N_DMA_SEMS
        key = ("d", eng, i)
        if self.dcnt[eng][i] > 0:
            self._wait(eng, (key, self.dcnt[eng][i]))
        self._deps(eng, reads, writes, extra)
        self.dcnt[eng][i] += 16
        val = self.dcnt[eng][i]
        sem = self.dsem[eng][i]
        self.q[eng].append(lambda E, fn=fn, sem=sem: fn(E).then_inc(sem, 16))
        tok = (key, val)
        self._commit(tok, reads, writes)
        return tok

    def cc(self, fn, reads=(), writes=(), extra=(), bg=False):
        eng = "pool"
        self._deps(eng, reads, writes, extra)
        self.cccnt += 1
        val = self.cccnt
        if not bg:
            self.ccfg = val
        sem = self.ccsem
        self.q[eng].append(lambda E, fn=fn, sem=sem: fn(E).then_inc(sem, 1))
        tok = (("cc",), val)
        self._commit(tok, reads, writes)
        return tok

    def all_tokens(self):
        toks = [(("e", e), self.cnt[e]) for e in ENGS if self.cnt[e] > 0]
        for e in self.dsem:
            for i in range(self.N_DMA_SEMS):
                if self.dcnt[e][i] > 0:
                    toks.append((("d", e, i), self.dcnt[e][i]))
        if self.ccfg > 0:
            toks.append((("cc",), self.ccfg))
        return toks

    def final_tokens(self):
        toks = self.all_tokens()
        for i in range(self.NBG):
            if self.bgcnt[i] > 0:
                toks.append((("bg", i), self.bgcnt[i]))
        if self.cccnt > 0:
            toks.append((("cc",), self.cccnt))
        return toks

    def barrier(self, engs=ENGS):
        toks = self.all_tokens()
        for e in engs:
            for t in toks:
                self._wait(e, t)

    def emit(self):
        with self.nc.Block() as block:
            @block.tensor
            def _(E):
                for f in self.q["pe"]:
                    f(E)

            @block.scalar
            def _(E):
                for f in self.q["act"]:
                    f(E)

            @block.vector
            def _(E):
                for f in self.q["dve"]:
                    f(E)

            @block.gpsimd
            def _(E):
                for f in self.q["pool"]:
                    f(E)

            @block.sync
            def _(E):
                for f in self.q["sp"]:
                    f(E)


BROWS = 3 * NL * 16 * 128 * 1536 // 2048
SEG_ROWS = {"w_in": 4096, "w_nao": 1024, "w_fo": 1024, "w_bg": 4096, "w_out": 2048,
            "w_gate": 16384, "w_up": 16384, "w_down": 16384}
STAGE_NEEDS = {
    "n1": [], "p1": ["w_in"], "halo": ["w_in"], "na": ["w_in"], "fourier": ["w_in"],
    "merge": ["w_in", "w_nao", "w_fo", "w_bg"], "mix": ["w_in", "w_nao", "w_fo", "w_bg", "w_out"],
    "route": ["w_in", "w_nao", "w_fo", "w_bg", "w_out"], "route2": ["w_in", "w_nao", "w_fo", "w_bg", "w_out"],
}


def seg_plan(nl, stop):
    names = list(SEG_ROWS)
    out_ = []
    for l in range(nl):
        if stop is not None and l == nl - 1 and stop in STAGE_NEEDS:
            use = STAGE_NEEDS[stop]
        else:
            use = names
        for n in names:
            if n in use:
                out_.append((n, l, SEG_ROWS[n]))
    return out_


def build(nl=NL, stop=None, dbg=()):
    nc = bass.Bass("TRN2", target_bir_lowering=False)

    def din(name, shape, dt=F32):
        return nc.dram_tensor(name, list(shape), dt, kind="ExternalInput").ap()

    def dscr(name, shape, dt=F32):
        return nc.dram_tensor(name, list(shape), dt).ap()

    def dout(name, shape, dt=F32):
        return nc.dram_tensor(name, list(shape), dt, kind="ExternalOutput").ap()

    x_in = din("x", [T, D])
    cT_in = din("cT", [128, 16])
    n1g_in = din("n1g", [NL, D])
    n2g_in = din("n2g", [NL, D])
    fing_in = din("fing", [1, D])
    adaw_in = din("adaw", [NL, D, 1536])
    adab_in = din("adab", [1, NL * 1536])
    sel_in = din("selrow", [1, 128])
    segs = seg_plan(nl, stop)
    WROWS = sum(r for _, _, r in segs)
    NOWN = WROWS // NCORE
    NWT = NOWN // 128
    woff = {}
    o_ = 0
    for nm_, l_, r_ in segs:
        woff[(nm_, l_)] = o_
        o_ += r_
    wshard_in = din("wshard", [max(NOWN, 128), 2048])
    witab_in = din("witab", [128, max(NWT, 1)], I32)
    bshard_in = din("bshard", [BROWS // NCORE, 2048])
    bidx_in = din("bidx", [128, NL * 16 * 3 + BROWS // NCORE // 128], I32)
    bbg_in = din("b_bgT", [NL, 128, 32])
    wr_in = din("w_r", [NL, D, 36])
    br_in = din("b_r", [NL, 36])
    cdft_in = din("cdft", [128, 2, 512], BF16)
    csl_in = din("csl", [8192, 1024], BF16)
    ssl_in = din("ssl", [8192, 1024], BF16)
    ident_in = din("ident", [128, 128], BF16)
    tri_in = din("tri", [128, 256], BF16)
    itab_in = din("itab", [128, 24], I32)
    flg_in = din("flags", [128, 2])
    eslot_in = din("eslot", [128, NE + 1])
    out = dout("out", [T, D])
    dbg_out = {}

    xres = dscr("xres", [T, D])
    modin = dscr("modin", [32, 1536])
    modall = dscr("modall", [32, 1536])
    ka_in = dscr("ka_in", [NCORE * 128, 2048], BF16)
    ka_all = dscr("ka_all", [NCORE * 128, 2048], BF16)
    kb_in = dscr("kb_in", [NCORE * 128, 2048], BF16)
    kb_all = dscr("kb_all", [NCORE * 128, 2048], BF16)
    va_in = dscr("va_in", [NCORE * 256, 1024], BF16)
    va_all = dscr("va_all", [NCORE * 256, 1024], BF16)
    vb_in = dscr("vb_in", [NCORE * 256, 1024], BF16)
    vb_all = dscr("vb_all", [NCORE * 256, 1024], BF16)
    ab_in = dscr("ab_in", [NCORE * T, 2048], BF16)
    ab_all = dscr("ab_all", [NCORE * T, 2048], BF16)
    seg_sizes = sorted({r_ for _, _, r_ in segs})
    NIB = {r_: (3 if r_ >= 16384 else 2) for r_ in seg_sizes}
    inbuf = {r_: [dscr(f"wallin_{r_}_{k_}", [r_, 2048], BF16) for k_ in range(NIB[r_])] for r_ in seg_sizes}
    toff = {}
    t_ = 0
    for n_, l_, r_ in segs:
        toff[(n_, l_)] = t_
        t_ += r_ // NCORE // 128
    wall_t = {(n_, l_): dscr(f"wall_{n_}_{l_}", [r_, 2048], BF16) for n_, l_, r_ in segs}
    ball_in = dscr("ball_in", [BROWS, 2048], BF16)
    ball = dscr("ball", [BROWS, 2048], BF16)
    ynad = dscr("ynad", [T, 1024], BF16)
    xg = dscr("xg", [NE * CAP + 128, D], BF16)
    yg = dscr("yg", [NE * CAP + 128, D], BF16)

    RG = [list(range(NCORE))]

    def wview(name, l, e=None):
        o = 0
        wall = wall_t[(name, l)]
        if name in ("w_in", "w_bg"):
            return wall[o:o + 4096, :].rearrange("(r h) c -> r (h c)", h=2)
        if name in ("w_nao", "w_fo"):
            return wall[o:o + 1024, :]
        if name == "w_out":
            return wall[o:o + 2048, :]
        if name in ("w_gate", "w_up"):
            return wall[o + e * 512:o + (e + 1) * 512, :].rearrange("r (q f) -> (r q) f", q=4)
        if name == "w_down":
            return wall[o + e * 512:o + (e + 1) * 512, :]
        raise KeyError(name)

    ballv = ball.rearrange("r c -> (r c)").rearrange("(n f) -> n f", f=1536)

    with ExitStack() as es:
        S = Sched(nc, es)

        uid = [0]

        def alloc(st, name, shape, dt):
            uid[0] += 1
            return st.enter_context(nc.sbuf_tensor(f"sb_{name}_{uid[0]}", list(shape), dt))

        ps = [es.enter_context(nc.psum_tensor(f"ps{i}", [128, 512], F32)) for i in range(8)]
        psB = [Buf(f"ps{i}") for i in range(8)]
        psn = [0]

        def nextps():
            i = psn[0]
            psn[0] = (i + 1) % 8
            return i

        ident = alloc(es, "ident", [128, 128], BF16)
        tri = alloc(es, "tri", [128, 256], BF16)
        cdft = alloc(es, "cdft", [128, 2, 512], BF16)
        itab = alloc(es, "itab", [128, 24], I32)
        flg = alloc(es, "flg", [128, 2], F32)
        eslot = alloc(es, "eslot", [128, NE + 1], F32)
        epsT = alloc(es, "epsT", [128, 1], F32)
        Bconst = Buf("const")
        for dst, src in ((ident, ident_in), (tri, tri_in), (cdft, cdft_in), (itab, itab_in), (flg, flg_in),
                         (eslot, eslot_in)):
            S.dma("sp", lambda E, dst=dst, src=src: E.dma_start(out=dst[:], in_=src), writes=[Bconst])
        S.op("dve", lambda E: E.memset(epsT[:], 1e-6), writes=[Bconst])
        Bx = [Buf(f"xres{i}") for i in range(8)]
        Bdram = {n: Buf(n) for n in ("modin", "modall", "ka_in", "ka_all", "kb_in", "kb_all", "va_in", "va_all",
                                     "vb_in", "vb_all", "ab_in", "ab_all", "xg", "yg", "out", "ynad")}

        def idx(col, rows=128):
            return bass.IndirectOffsetOnAxis(itab[0:rows, col:col + 1], 0)

        witab = alloc(es, "witab", [128, max(NWT, 1)], I32)
        bidx = alloc(es, "bidx", [128, NL * 16 * 3 + BROWS // NCORE // 128], I32)
        S.dma("sp", lambda E: E.dma_start(out=witab[:], in_=witab_in), writes=[Bconst])
        S.dma("sp", lambda E: E.dma_start(out=bidx[:], in_=bidx_in), writes=[Bconst])
        Bdram["wall_in"] = Buf("wall_in")
        Bdram["wall"] = Buf("wall")
        Bdram["ball_in"] = Buf("ball_in")
        Bdram["ball"] = Buf("ball")
        with ExitStack() as ph:
            z = alloc(ph, "zt", [128, 16384], BF16)
            Bz = Buf("z")
            S.op("pool", lambda E: E.memset(z[:], 0.0), writes=[Bz])
            zlist = [("ka_in", ka_in, 1024, 2048), ("kb_in", kb_in, 1024, 2048), ("va_in", va_in, 2048, 1024),
                     ("vb_in", vb_in, 2048, 1024), ("ab_in", ab_in, 8192, 2048), ("ball_in", ball_in, BROWS, 2048)]
            for r_ in seg_sizes:
                for k_ in range(NIB[r_]):
                    zlist.append(("wall_in", inbuf[r_][k_], r_, 2048))
            qi = 0
            for name, ap_, rows, cols in zlist:
                per = rows // 128
                chunk = max(1, 16384 // cols)
                for r0 in range(0, per, chunk):
                    n = min(chunk, per - r0)
                    S.dma(("sp", "act")[qi % 2], lambda E, ap_=ap_, r0=r0, n=n, cols=cols: E.dma_start(
                        out=ap_.rearrange("(p r) c -> p r c", p=128)[:, r0:r0 + n, :],
                        in_=z[:, 0:n * cols].rearrange("p (r c) -> p r c", c=cols)),
                        reads=[Bz], writes=[Bdram[name]])
                    qi += 1
            S.barrier()
        with ExitStack() as ph:
            wst = [alloc(ph, f"wst{i}", [128, 2048], BF16) for i in range(4)]
            Bwst = [Buf() for _ in range(4)]
            NBT = BROWS // NCORE // 128
            for t in range(NBT):
                w_, Bw_ = wst[t % 4], Bwst[t % 4]
                S.dma("pool", lambda E, w_=w_, t=t: E.dma_start(out=w_[:], in_=bshard_in[t * 128:(t + 1) * 128, :]), writes=[Bw_])
                S.dma("pool", lambda E, w_=w_, t=t: E.indirect_dma_start(
                    out=ball_in, out_offset=bass.IndirectOffsetOnAxis(bidx[:, NL * 48 + t:NL * 48 + t + 1], 0), in_=w_[:], in_offset=None),
                    reads=[Bw_, Bconst], writes=[Bdram["ball_in"]])
            S.cc(lambda E: E.collective_compute("AllReduce", ALU.add, replica_groups=RG, ins=[ball_in.opt()], outs=[ball.opt()]),
                 reads=[Bdram["ball_in"]], writes=[Bdram["ball"]])
            S.barrier()

        wstg = [alloc(es, f"wstg{i}", [128, 2048], BF16) for i in range(4)]
        Bwstg = [Buf() for _ in range(4)]
        Binbuf = {r_: [Buf() for _ in range(NIB[r_])] for r_ in seg_sizes}
        inuse = {r_: 0 for r_ in seg_sizes}
        Bwall = {(n_, l_): Buf(f"wall_{n_}_{l_}") for n_, l_, r_ in segs}
        dq = []
        dstate = {"cast": 0, "scat": 0}

        def dist_enqueue(seglist):
            for n_, l_, r_ in seglist:
                k_ = inuse[r_] % NIB[r_]
                inuse[r_] += 1
                nt = r_ // NCORE // 128
                for j_ in range(nt):
                    dq.append(("tile", toff[(n_, l_)] + j_, r_, k_, None))
                dq.append(("ar", None, r_, k_, (n_, l_)))

        def _dist_cast(i, bg):
            kind, t, r_, k_, _ = dq[i]
            if kind != "tile":
                return
            slot = dstate["cast"] % 4
            dstate["cast"] += 1
            w_ = wstg[slot]
            dq[i] = (kind, t, r_, k_, slot)
            S.dma("pool", lambda E, w_=w_, t=t: E.dma_start(out=w_[:], in_=wshard_in[t * 128:(t + 1) * 128, :]),
                  writes=[Bwstg[slot]], bg=bg)

        dpos = {"cast": 0, "done": 0}

        def dist_pump(n, bg=True):
            for _ in range(n):
                if dpos["done"] >= len(dq):
                    return
                while dpos["cast"] < len(dq) and dpos["cast"] < dpos["done"] + 3:
                    _dist_cast(dpos["cast"], bg)
                    dpos["cast"] += 1
                kind, t, r_, k_, x_ = dq[dpos["done"]]
                dpos["done"] += 1
                ib = inbuf[r_][k_]
                if kind == "tile":
                    slot = x_
                    S.dma("pool", lambda E, ib=ib, t=t, slot=slot: E.indirect_dma_start(
                        out=ib, out_offset=bass.IndirectOffsetOnAxis(witab[:, t:t + 1], 0), in_=wstg[slot][:], in_offset=None),
                        reads=[Bwstg[slot], Bconst], writes=[Binbuf[r_][k_]], bg=bg)
                else:
                    S.cc(lambda E, ib=ib, wo_=wall_t[x_]: E.collective_compute("AllReduce", ALU.add, replica_groups=RG,
                                                                               ins=[ib.opt()], outs=[wo_.opt()]),
                         reads=[Binbuf[r_][k_]], writes=[Bwall[x_]], bg=bg)

        def dist_flush(bg=True):
            dist_pump(len(dq) + 1, bg)

        MIXN = ("w_in", "w_nao", "w_fo", "w_bg", "w_out")
        dist_enqueue([sg for sg in segs if sg[1] == 0 and sg[0] in MIXN])
        dist_flush(bg=False)
        S.barrier()

        with ExitStack() as ph:
            ct = alloc(ph, "ct", [128, 16], F32)
            condT = alloc(ph, "condT", [128, 16], BF16)
            wt = [alloc(ph, f"adw{i}", [128, 16, 512], BF16) for i in range(2)]
            Bwt = [Buf(), Buf()]
            modrow = alloc(ph, "modrow", [1, NL * 1536], F32)
            adabt = alloc(ph, "adabt", [1, NL * 1536], F32)
            selt = alloc(ph, "selt", [1, 128], F32)
            modp = alloc(ph, "modp", [32, 1536], F32)
            Bc, Bmr, Bmp = Buf(), Buf(), Buf()
            S.dma("sp", lambda E: E.dma_start(out=ct[:], in_=cT_in), writes=[Bc])
            S.dma("sp", lambda E: E.dma_start(out=adabt[:], in_=adab_in), writes=[Bc])
            S.dma("sp", lambda E: E.dma_start(out=selt[:], in_=sel_in), writes=[Bc])
            S.op("act", lambda E: E.activation(out=condT[:], in_=ct[:], func=AF.Silu), reads=[Bc], writes=[Bc])
            k = 0
            for l in range(NL):
                for nb in range(3):
                    w = wt[k % 2]
                    S.dma("pool", lambda E, w=w, l=l, nb=nb: E.dma_start(
                        out=w[:], in_=adaw_in[l, :, nb * 512:(nb + 1) * 512].rearrange("(kc p) n -> p kc n", p=128)),
                        writes=[Bwt[k % 2]])
                    pi = nextps()

                    def mmf(E, w=w, pi=pi):
                        r = None
                        for kc in range(16):
                            r = E.matmul(ps[pi][0:1, :], condT[:, kc:kc + 1], w[:, kc, :], start=(kc == 0), stop=(kc == 15))
                        return r
                    S.op("pe", mmf, reads=[Bwt[k % 2], Bc], writes=[psB[pi]])
                    o = l * 1536 + nb * 512
                    S.op("dve", lambda E, pi=pi, o=o: E.tensor_tensor(out=modrow[0:1, o:o + 512], in0=ps[pi][0:1, :],
                                                                       in1=adabt[0:1, o:o + 512], op=ALU.add),
                         reads=[psB[pi], Bc], writes=[Bmr])
                    k += 1
            for nb in range(3):
                pi = nextps()

                def plc(E, pi=pi, nb=nb):
                    r = None
                    for l in range(NL):
                        o = l * 1536 + nb * 512
                        r = E.matmul(ps[pi][0:32, :], selt[0:1, l * 32:(l + 1) * 32], modrow[0:1, o:o + 512],
                                     start=(l == 0), stop=(l == NL - 1))
                    return r
                S.op("pe", plc, reads=[Bmr, Bc], writes=[psB[pi]])
                S.op("act", lambda E, pi=pi, nb=nb: E.activation(out=modp[:, nb * 512:(nb + 1) * 512], in_=ps[pi][0:32, :],
                                                                func=AF.Copy), reads=[psB[pi]], writes=[Bmp])
            S.dma("sp", lambda E: E.dma_start(out=modin, in_=modp[:]), reads=[Bmp], writes=[Bdram["modin"]])
            S.cc(lambda E: E.collective_compute("AllReduce", ALU.add, replica_groups=RG, ins=[modin.opt()],
                                                outs=[modall.opt()]),
                 reads=[Bdram["modin"]], writes=[Bdram["modall"]])
            S.barrier()
        if "mod" in dbg:
            dbg_out["mod"] = dout("dbg_mod", [32, 1536])
            with ExitStack() as ph:
                t = alloc(ph, "dbgm", [32, 1536], F32)
                Bt = Buf()
                S.dma("sp", lambda E: E.dma_start(out=t[:], in_=modall), reads=[Bdram["modall"]], writes=[Bt])
                S.dma("sp", lambda E: E.dma_start(out=dbg_out["mod"], in_=t[:]), reads=[Bt], writes=[Bdram["out"]])
                S.barrier()

        modv = modall.rearrange("(c l) (j w) -> l j c w", l=NL, w=256)

        def load_bcast_mod(tile_, l, j, B):
            return S.dma("sp", lambda E: E.dma_start(out=tile_[:].rearrange("p (c w) -> p c w", c=8),
                                                     in_=modv[l, j].partition_broadcast(128)),
                         reads=[Bdram["modall"]], writes=[B])

        def load_bcast_row(tile_, row_ap, B):
            return S.dma("sp", lambda E: E.dma_start(out=tile_[:], in_=row_ap.partition_broadcast(128)), writes=[B])

        def phase_norm(ph, l, gain_ap, jsh, jsc, xsrc, Bxsrc, hT, BhT, tile_hook=None, post_hook=None, final=False):
            Gb = alloc(ph, "Gb", [128, D], F32)
            SHb = alloc(ph, "SHb", [128, D], F32)
            tmpb = alloc(ph, "tmpb", [128, D], F32)
            BG = Buf()
            load_bcast_row(Gb, gain_ap, BG)
            if not final:
                load_bcast_mod(tmpb, l, jsc, BG)
                load_bcast_mod(SHb, l, jsh, BG)
                S.op("dve", lambda E: E.scalar_tensor_tensor(out=Gb[:], in0=tmpb[:], scalar=1.0, in1=Gb[:],
                                                             op0=ALU.add, op1=ALU.mult), reads=[BG], writes=[BG])
            xt = [alloc(ph, f"xt{i}", [128, D], F32) for i in range(2)]
            Bxt = [Buf(), Buf()]
            sq = alloc(ph, "sq", [128, D], BF16)
            Bsq = Buf()
            ss = alloc(ph, "ss", [128, 8], F32)
            rs = alloc(ph, "rs", [128, 8], F32)
            hn = alloc(ph, "hn", [128, D], F32)
            Bhn = Buf()
            hb = [alloc(ph, f"hb{i}", [128, D], BF16) for i in range(2)]
            Bhb = [Buf(), Buf()]
            Bss = [Buf() for _ in range(8)]
            for tt in range(8):
                x_ = xt[tt % 2]
                Bx_ = Bxt[tt % 2]
                S.dma("sp", lambda E, x_=x_, tt=tt: E.dma_start(out=x_[:], in_=xsrc[tt * 128:(tt + 1) * 128, :]),
                      reads=[Bxsrc[tt]], writes=[Bx_])
                S.op("act", lambda E, x_=x_, tt=tt: E.activation(out=sq[:], in_=x_[:], func=AF.Square,
                                                                 accum_out=ss[:, tt:tt + 1]),
                     reads=[Bx_], writes=[Bsq, Bss[tt]])
                S.op("act", lambda E, tt=tt: E.activation(out=rs[:, tt:tt + 1], in_=ss[:, tt:tt + 1], func=AF.Sqrt,
                                                          scale=1.0 / D, bias=epsT[:, 0:1]),
                     reads=[Bss[tt], Bconst], writes=[Bss[tt]])
                S.op("dve", lambda E, tt=tt: E.reciprocal(out=rs[:, tt:tt + 1], in_=rs[:, tt:tt + 1]),
                     reads=[Bss[tt]], writes=[Bss[tt]])
                if final:
                    S.op("dve", lambda E, x_=x_, tt=tt: E.scalar_tensor_tensor(
                        out=hn[:], in0=x_[:], scalar=rs[:, tt:tt + 1], in1=Gb[:], op0=ALU.mult, op1=ALU.mult),
                        reads=[Bx_, Bss[tt], BG], writes=[Bhn])
                    S.dma("sp", lambda E, tt=tt: E.dma_start(out=out[tt * 128:(tt + 1) * 128, :], in_=hn[:]),
                          reads=[Bhn], writes=[Bdram["out"]])
                    continue
                S.op("dve", lambda E, x_=x_, tt=tt: E.scalar_tensor_tensor(
                    out=hn[:], in0=x_[:], scalar=rs[:, tt:tt + 1], in1=Gb[:], op0=ALU.mult, op1=ALU.mult),
                    reads=[Bx_, Bss[tt], BG], writes=[Bhn])
                h_ = hb[tt % 2]
                Bh_ = Bhb[tt % 2]
                S.op("dve", lambda E, h_=h_: E.tensor_tensor(out=h_[:], in0=hn[:], in1=SHb[:], op=ALU.add),
                     reads=[Bhn, BG], writes=[Bh_])
                if tile_hook is not None:
                    tile_hook(tt, h_, Bh_)
                for half in range(2):
                    pi = nextps()
                    pbf = ps[pi][:].bitcast(BF16)

                    def trf(E, h_=h_, pbf=pbf, half=half):
                        r = None
                        for j in range(8):
                            kc = half * 8 + j
                            r = E.transpose(out=pbf[:, j * 128:(j + 1) * 128], in_=h_[:, kc * 128:(kc + 1) * 128],
                                            identity=ident[:])
                        return r
                    S.op("pe", trf, reads=[Bh_, Bconst], writes=[psB[pi]])
                    eng = "act" if half == 0 else "dve"
                    dst = hT[:, half * 8:(half + 1) * 8, tt * 128:(tt + 1) * 128]
                    src = pbf.rearrange("p (k t) -> p k t", k=8)
                    if eng == "act":
                        S.op("act", lambda E, dst=dst, src=src: E.activation(out=dst, in_=src, func=AF.Copy),
                             reads=[psB[pi]], writes=[BhT[tt]])
                    else:
                        S.op("dve", lambda E, dst=dst, src=src: E.tensor_copy(out=dst, in_=src),
                             reads=[psB[pi]], writes=[BhT[tt]])
                if post_hook is not None:
                    post_hook(tt, h_, Bh_)

        def evac(i, out_ap, in_ap, reads, writes, scale=None):
            if i % 2 == 0:
                if scale is None:
                    return S.op("act", lambda E: E.activation(out=out_ap, in_=in_ap, func=AF.Copy), reads=reads, writes=writes)
                return S.op("act", lambda E: E.activation(out=out_ap, in_=in_ap, func=AF.Copy, scale=scale),
                            reads=reads, writes=writes)
            if scale is None:
                return S.op("dve", lambda E: E.tensor_copy(out=out_ap, in_=in_ap), reads=reads, writes=writes)
            return S.op("dve", lambda E: E.tensor_scalar(out=out_ap, in0=in_ap, scalar1=scale, scalar2=None, op0=ALU.mult),
                        reads=reads, writes=writes)

        def mm_group(pi, out_ap, pairs, reads):
            def fn(E):
                r = None
                n = len(pairs)
                for i, (lt, rt) in enumerate(pairs):
                    r = E.matmul(out_ap, lt, rt, start=(i == 0), stop=(i == n - 1))
                return r
            return S.op("pe", fn, reads=reads, writes=[psB[pi]])

        with ExitStack() as ph:
            t2 = [alloc(ph, f"cp{i}", [128, D], F32) for i in range(2)]
            Bt2 = [Buf(), Buf()]
            for tt in range(8):
                S.dma("sp", lambda E, tt=tt: E.dma_start(out=t2[tt % 2][:], in_=x_in[tt * 128:(tt + 1) * 128, :]),
                      writes=[Bt2[tt % 2]])
                S.dma("sp", lambda E, tt=tt: E.dma_start(out=xres[tt * 128:(tt + 1) * 128, :], in_=t2[tt % 2][:]),
                      reads=[Bt2[tt % 2]], writes=[Bx[tt]])
            S.barrier()

        def dump(name, shape, dt, src_fn):
            dbg_out[name] = dout("dbg_" + name, shape, dt)


        def mixer(l):
            with ExitStack() as mx:
                hT = alloc(mx, "hT", [128, 16, T], BF16)
                BhT = [Buf(f"hT{i}") for i in range(8)]
                with ExitStack() as ph:
                    phase_norm(ph, l, n1g_in[l:l + 1, :], 0, 1, xres, Bx, hT, BhT)
                    S.barrier()
                if stop == "n1":
                    dbg_out["hT"] = dout("dbg_hT", [128, 16 * T], BF16)
                    S.dma("sp", lambda E: E.dma_start(out=dbg_out["hT"], in_=hT[:].rearrange("p k t -> p (k t)")),
                          reads=BhT, writes=[Bdram["out"]])
                    return True
                ynaT_stack = ExitStack()
                with ExitStack() as qkv:
                    qT = alloc(qkv, "qT", [128, 8, T], BF16)
                    kT = alloc(qkv, "kT", [128, 8, 1536], BF16)
                    Vaug = alloc(qkv, "Vaug", [128, 12, 16, 65], BF16)
                    Bq, Bk, Bv, Byna = Buf("q"), Buf("k"), Buf("v"), Buf("yna")
                    S.op("pool", lambda E: E.memset(Vaug[:], 0.0), writes=[Bv])
                    S.op("pool", lambda E: E.memset(kT[:, :, 1280:1536], 0.0), writes=[Bk])
                    S.op("pool", lambda E: E.memset(Vaug[:, 2:10, :, 64:65], 1.0), writes=[Bv])
                    with ExitStack() as ph:
                        wb = [alloc(ph, f"wb{i}", [128, 16, 512], BF16) for i in range(2)]
                        Bwb = [Buf(), Buf()]
                        uT = alloc(ph, "uT", [128, 8, T], BF16)
                        Bu = Buf("u")
                        abt = [alloc(ph, f"abt{i}", [128, 2, 1024], BF16) for i in range(2)]
                        Babt = [Buf(), Buf()]
                        ec = 0
                        for blk in range(8):
                            w = wb[blk % 2]
                            Bw = Bwb[blk % 2]
                            S.dma("act", lambda E, w=w, blk=blk: E.dma_start(
                                out=w[:], in_=wview("w_in", l)[:, blk * 512:(blk + 1) * 512].rearrange("(kc p) n -> p kc n", p=128)),
                                reads=[Bwall[("w_in", l)]], writes=[Bw])
                            kind = blk // 2
                            if kind == 2:
                                for tt in range(8):
                                    pi = nextps()
                                    mm_group(pi, ps[pi][:, :], [(hT[:, kc, tt * 128:(tt + 1) * 128], w[:, kc, :]) for kc in range(16)],
                                             reads=[Bw, BhT[tt]])
                                    h0 = (blk % 2) * 8
                                    evac(ec, Vaug[:, 2 + tt, h0:h0 + 8, 0:64], ps[pi][:, :].rearrange("p (h d) -> p h d", h=8),
                                         reads=[psB[pi]], writes=[Bv])
                                    ec += 1
                                continue
                            for th in range(2):
                                for fc in range(4):
                                    pi = nextps()
                                    mm_group(pi, ps[pi][:, :], [(w[:, kc, fc * 128:(fc + 1) * 128], hT[:, kc, th * 512:(th + 1) * 512])
                                                                for kc in range(16)], reads=[Bw] + BhT[th * 4:(th + 1) * 4])
                                    ch = (blk % 2) * 4 + fc
                                    if kind == 0:
                                        evac(ec, qT[:, ch, th * 512:(th + 1) * 512], ps[pi][:, :], [psB[pi]], [Bq], scale=0.125)
                                    elif kind == 1:
                                        evac(ec, kT[:, ch, 256 + th * 512:256 + (th + 1) * 512], ps[pi][:, :], [psB[pi]], [Bk])
                                    else:
                                        evac(ec, uT[:, ch, th * 512:(th + 1) * 512], ps[pi][:, :], [psB[pi]], [Bu])
                                    ec += 1
                        for tt in range(8):
                            a_ = abt[tt % 2]
                            Ba_ = Babt[tt % 2]
                            for g in range(4):
                                pi = nextps()
                                mm_group(pi, ps[pi][:, :], [(uT[:, 2 * g + mc, tt * 128:(tt + 1) * 128], cdft[:, mc, :]) for mc in range(2)],
                                         reads=[Bu, Bconst])
                                evac(ec, a_[:, :, g * 256:(g + 1) * 256], ps[pi][:, :].rearrange("p (a j) -> p a j", a=2),
                                     [psB[pi]], [Ba_], scale=1.0 / 16.0)
                                ec += 1
                            S.dma("pool", lambda E, a_=a_, tt=tt: E.indirect_dma_start(
                                out=ab_in, out_offset=idx(tt), in_=a_[:].rearrange("p a j -> p (a j)"), in_offset=None),
                                reads=[Ba_, Bconst], writes=[Bdram["ab_in"]])
                        kst = alloc(ph, "kst", [128, 8, 256], BF16)
                        kst2 = alloc(ph, "kst2", [128, 8, 256], BF16)
                        Bks = Buf()
                        S.op("act", lambda E: E.activation(out=kst[:], in_=kT[:, :, 1024:1280], func=AF.Copy), reads=[Bk], writes=[Bks])
                        S.op("dve", lambda E: E.tensor_copy(out=kst2[:], in_=kT[:, :, 256:512]), reads=[Bk], writes=[Bks])
                        S.dma("pool", lambda E: E.indirect_dma_start(out=ka_in, out_offset=idx(8), in_=kst[:].rearrange("p a t -> p (a t)"),
                                                                     in_offset=None), reads=[Bks, Bconst], writes=[Bdram["ka_in"]])
                        S.dma("pool", lambda E: E.indirect_dma_start(out=kb_in, out_offset=idx(8), in_=kst2[:].rearrange("p a t -> p (a t)"),
                                                                     in_offset=None), reads=[Bks, Bconst], writes=[Bdram["kb_in"]])
                        vst = alloc(ph, "vst", [128, 4, 1024], BF16)
                        Bvs = Buf()
                        for i, chn in enumerate((8, 9, 2, 3)):
                            evac(i, vst[:, i, :].rearrange("p (h d) -> p h d", h=16), Vaug[:, chn, :, 0:64], [Bv], [Bvs])
                        S.dma("pool", lambda E: E.indirect_dma_start(out=va_in, out_offset=idx(9), in_=vst[:, 0, :], in_offset=None),
                              reads=[Bvs, Bconst], writes=[Bdram["va_in"]])
                        S.dma("pool", lambda E: E.indirect_dma_start(out=va_in, out_offset=idx(10), in_=vst[:, 1, :], in_offset=None),
                              reads=[Bvs, Bconst], writes=[Bdram["va_in"]])
                        S.dma("pool", lambda E: E.indirect_dma_start(out=vb_in, out_offset=idx(11), in_=vst[:, 2, :], in_offset=None),
                              reads=[Bvs, Bconst], writes=[Bdram["vb_in"]])
                        S.dma("pool", lambda E: E.indirect_dma_start(out=vb_in, out_offset=idx(12), in_=vst[:, 3, :], in_offset=None),
                              reads=[Bvs, Bconst], writes=[Bdram["vb_in"]])
                        for nm, a_i, a_o in (("ka", ka_in, ka_all), ("kb", kb_in, kb_all), ("va", va_in, va_all), ("vb", vb_in, vb_all),
                                             ("ab", ab_in, ab_all)):
                            S.cc(lambda E, a_i=a_i, a_o=a_o: E.collective_compute("AllReduce", ALU.add, replica_groups=RG,
                                                                                  ins=[a_i.opt()], outs=[a_o.opt()]),
                                 reads=[Bdram[nm + "_in"]], writes=[Bdram[nm + "_all"]])
                        S.barrier()
                        if l == 0:
                            dist_enqueue([sg for sg in segs if sg[1] == l and sg[0] not in MIXN])
                    def recv_halos():
                        with ExitStack() as ph:
                            st = alloc(ph, "hst", [128, 2048], BF16)
                            st2 = alloc(ph, "hst2", [128, 2048], BF16)
                            vs = alloc(ph, "hvs", [128, 4, 1024], BF16)
                            Bst, Bst2, Bvs2 = Buf(), Buf(), Buf()
                            S.dma("pool", lambda E: E.indirect_dma_start(out=st[:], out_offset=None, in_=ka_all, in_offset=idx(13)),
                                  reads=[Bdram["ka_all"], Bconst], writes=[Bst])
                            S.dma("pool", lambda E: E.indirect_dma_start(out=st2[:], out_offset=None, in_=kb_all, in_offset=idx(14)),
                                  reads=[Bdram["kb_all"], Bconst], writes=[Bst2])
                            S.op("act", lambda E: E.activation(out=kT[:, :, 0:256], in_=st[:].rearrange("p (a t) -> p a t", a=8), func=AF.Copy),
                                 reads=[Bst], writes=[Bk])
                            S.op("dve", lambda E: E.tensor_copy(out=kT[:, :, 1280:1536], in_=st2[:].rearrange("p (a t) -> p a t", a=8)),
                                 reads=[Bst2], writes=[Bk])
                            S.dma("pool", lambda E: E.indirect_dma_start(out=vs[:, 0, :], out_offset=None, in_=va_all, in_offset=idx(15)),
                                  reads=[Bdram["va_all"], Bconst], writes=[Bvs2])
                            S.dma("pool", lambda E: E.indirect_dma_start(out=vs[:, 1, :], out_offset=None, in_=va_all, in_offset=idx(16)),
                                  reads=[Bdram["va_all"], Bconst], writes=[Bvs2])
                            S.dma("pool", lambda E: E.indirect_dma_start(out=vs[:, 2, :], out_offset=None, in_=vb_all, in_offset=idx(17)),
                                  reads=[Bdram["vb_all"], Bconst], writes=[Bvs2])
                            S.dma("pool", lambda E: E.indirect_dma_start(out=vs[:, 3, :], out_offset=None, in_=vb_all, in_offset=idx(18)),
                                  reads=[Bdram["vb_all"], Bconst], writes=[Bvs2])
                            for i, (chn, fcol, rows) in enumerate(((0, 0, 128), (1, 0, 128), (10, 1, 128), (11, 1, 128))):
                                S.op("dve", lambda E, i=i, chn=chn, fcol=fcol, rows=rows: E.tensor_scalar(
                                    out=Vaug[0:rows, chn, :, 0:64], in0=vs[0:rows, i, :].rearrange("p (h d) -> p h d", h=16),
                                    scalar1=flg[0:rows, fcol:fcol + 1], scalar2=None, op0=ALU.mult), reads=[Bvs2, Bconst], writes=[Bv])
                                S.op("dve", lambda E, chn=chn, fcol=fcol, rows=rows: E.tensor_scalar(
                                    out=Vaug[0:rows, chn, :, 64:65], in0=Vaug[0:rows, 4, :, 64:65],
                                    scalar1=flg[0:rows, fcol:fcol + 1], scalar2=None, op0=ALU.mult), reads=[Bconst, Bv], writes=[Bv])
                            S.op("dve", lambda E: E.memset(Vaug[64:128, 11, :, :], 0.0), writes=[Bv])
                            S.barrier()

                    if stop in ("p1", "halo"):
                        if stop == "halo":
                            recv_halos()
                        for nm, tl, shp in (("qT", qT, [128, 8 * T]), ("kT", kT, [128, 8 * 1536]), ("V", Vaug, [128, 12 * 16 * 65])):
                            dbg_out[nm] = dout("dbg_" + nm, shp, BF16)
                            fl = {"qT": "p a t -> p (a t)", "kT": "p a t -> p (a t)", "V": "p c h d -> p (c h d)"}[nm]
                            S.dma("sp", lambda E, nm=nm, tl=tl, fl=fl: E.dma_start(out=dbg_out[nm], in_=tl[:].rearrange(fl)),
                                  reads=[Bq, Bk, Bv], writes=[Bdram["out"]])
                        dbg_out["ab"] = dout("dbg_ab", [128, 2048], BF16)
                        with ExitStack() as ph:
                            t_ = alloc(ph, "dbgab", [128, 2048], BF16)
                            Bt_ = Buf()
                            S.dma("sp", lambda E: E.dma_start(out=t_[:], in_=ab_all[3 * 1024 + 256:3 * 1024 + 384, :]),
                                  reads=[Bdram["ab_all"]], writes=[Bt_])
                            S.dma("sp", lambda E: E.dma_start(out=dbg_out["ab"], in_=t_[:]), reads=[Bt_], writes=[Bdram["out"]])
                            S.barrier()
                        return True
                    recv_halos()
                    with ExitStack() as ph:
                        yna = alloc(ph, "yna", [128, 8, 1024], BF16)
                        bt = [alloc(ph, f"bt{i}", [128, 3, 1536], BF16) for i in range(2)]
                        Bbt = [Buf(), Buf()]
                        sb = [alloc(ph, f"sb{i}", [128, 512], F32) for i in range(3)]
                        Bsb = [Buf() for _ in range(3)]
                        PT = [alloc(ph, f"PT{i}", [128, 6, 256], BF16) for i in range(2)]
                        BPT = [Buf(), Buf()]
                        rc = alloc(ph, "rc", [128, 8], F32)
                        Brc = [Buf() for _ in range(8)]
                        u_ = 0
                        rci = 0
                        for h in range(16):
                            hp, hh = h // 2, h % 2
                            pb = 64 * hh
                            b_ = bt[h % 2]
                            Bb_ = Bbt[h % 2]
                            for k3 in range(3):
                                S.dma("pool", lambda E, b_=b_, h=h, k3=k3: E.indirect_dma_start(
                                    out=b_[:, k3, :], out_offset=None, in_=ballv,
                                    in_offset=bass.IndirectOffsetOnAxis(bidx[:, (l * 16 + h) * 3 + k3:(l * 16 + h) * 3 + k3 + 1], 0)),
                                    reads=[Bdram["ball"], Bconst], writes=[Bb_])
                            dist_pump(26)
                            for b in range(4):
                                cls = 0 if b == 0 else (2 if b == 3 else 1)
                                P_ = PT[u_ % 2]
                                BP_ = BPT[u_ % 2]
                                u_ += 1
                                for k2 in range(3):
                                    pi = nextps()

                                    def sfn(E, pi=pi, k2=k2, pb=pb, hp=hp, b=b):
                                        r = None
                                        for j in range(2):
                                            kc = 2 * k2 + j
                                            r = E.matmul(ps[pi][:, j * 256:(j + 1) * 256],
                                                         kT[pb:pb + 64, hp, (2 * b + kc) * 128:(2 * b + kc + 1) * 128],
                                                         qT[pb:pb + 64, hp, b * 256:(b + 1) * 256], start=True, stop=True)
                                        return r
                                    S.op("pe", sfn, reads=[Bk, Bq], writes=[psB[pi]])
                                    s_ = sb[k2]
                                    S.op("dve", lambda E, pi=pi, s_=s_, b_=b_, cls=cls, k2=k2: E.tensor_tensor(
                                        out=s_[:], in0=ps[pi][:, :], in1=b_[:, cls, k2 * 512:(k2 + 1) * 512], op=ALU.add),
                                        reads=[psB[pi], Bb_], writes=[Bsb[k2]])
                                    S.op("act", lambda E, s_=s_, P_=P_, k2=k2: E.activation(
                                        out=P_[:, 2 * k2:2 * k2 + 2, :].rearrange("p a q -> p (a q)"), in_=s_[:], func=AF.Exp),
                                        reads=[Bsb[k2]], writes=[BP_])
                                for qt in range(2):
                                    pi = nextps()

                                    def pvf(E, pi=pi, qt=qt, P_=P_, b=b, h=h):
                                        r = None
                                        for kc in range(6):
                                            r = E.matmul(ps[pi][:, 0:65], P_[:, kc, qt * 128:(qt + 1) * 128], Vaug[:, 2 * b + kc, h, :],
                                                         start=(kc == 0), stop=(kc == 5))
                                        return r
                                    S.op("pe", pvf, reads=[BP_, Bv], writes=[psB[pi]])
                                    r_ = rci % 8
                                    rci += 1
                                    S.op("dve", lambda E, pi=pi, r_=r_: E.reciprocal(out=rc[:, r_:r_ + 1], in_=ps[pi][:, 64:65]),
                                         reads=[psB[pi]], writes=[Brc[r_]])
                                    S.op("act", lambda E, pi=pi, r_=r_, b=b, qt=qt, h=h: E.activation(
                                        out=yna[:, 2 * b + qt, h * 64:(h + 1) * 64], in_=ps[pi][:, 0:64], func=AF.Copy,
                                        scale=rc[:, r_:r_ + 1]), reads=[psB[pi], Brc[r_]], writes=[Byna])
                        S.dma("sp", lambda E: E.dma_start(out=ynad.rearrange("(a p) f -> p a f", p=128), in_=yna[:]),
                              reads=[Byna], writes=[Bdram["ynad"]])
                        if stop == "na":
                            dbg_out["yna"] = dout("dbg_yna", [128, 8 * 1024], BF16)
                            S.dma("sp", lambda E: E.dma_start(out=dbg_out["yna"], in_=yna[:].rearrange("p a t -> p (a t)")),
                                  reads=[Byna], writes=[Bdram["out"]])
                        S.barrier()
                    if stop == "na":
                        return True
                with ynaT_stack:
                    ynaT = alloc(ynaT_stack, "ynaT", [128, 8, T], BF16)
                    BynaT = Buf("ynaT")
                    with ExitStack() as ph:
                        yt = [alloc(ph, f"yt{i}", [128, 1024], BF16) for i in range(2)]
                        Byt = [Buf(), Buf()]
                        for tt in range(8):
                            y_ = yt[tt % 2]
                            S.dma("sp", lambda E, y_=y_, tt=tt: E.dma_start(out=y_[:], in_=ynad[tt * 128:(tt + 1) * 128, :]),
                                  reads=[Bdram["ynad"]], writes=[Byt[tt % 2]])
                            pi = nextps()
                            pbf = ps[pi][:].bitcast(BF16)

                            def trf(E, pbf=pbf, y_=y_):
                                r = None
                                for j in range(8):
                                    r = E.transpose(out=pbf[:, j * 128:(j + 1) * 128], in_=y_[:, j * 128:(j + 1) * 128], identity=ident[:])
                                return r
                            S.op("pe", trf, reads=[Byt[tt % 2], Bconst], writes=[psB[pi]])
                            evac(tt, ynaT[:, :, tt * 128:(tt + 1) * 128], pbf.rearrange("p (k t) -> p k t", k=8), [psB[pi]], [BynaT])
                        S.barrier()
                    yfT = alloc(ynaT_stack, "yfT", [128, 8, T], BF16)
                    ByfT = Buf("yfT")
                    with ExitStack() as ph:
                        ab = [alloc(ph, f"fab{i}", [128, 2048], BF16) for i in range(3)]
                        cs = [alloc(ph, f"fcs{i}", [128, 2, 512], BF16) for i in range(3)]
                        Bab = [Buf() for _ in range(3)]
                        Bcs = [Buf() for _ in range(3)]
                        n_ = 0
                        for kh in range(2):
                            for sc in range(64):
                                a_, c_ = ab[n_ % 3], cs[n_ % 3]
                                Ba_, Bc_ = Bab[n_ % 3], Bcs[n_ % 3]
                                n_ += 1
                                S.dma("sp", lambda E, a_=a_, sc=sc: E.dma_start(out=a_[:], in_=ab_all[sc * 128:(sc + 1) * 128, :]),
                                      reads=[Bdram["ab_all"]], writes=[Ba_])
                                S.dma("act", lambda E, c_=c_, sc=sc, kh=kh: E.dma_start(
                                    out=c_[:, 0, :], in_=csl_in[sc * 128:(sc + 1) * 128, kh * 512:(kh + 1) * 512]), writes=[Bc_])
                                S.dma("act", lambda E, c_=c_, sc=sc, kh=kh: E.dma_start(
                                    out=c_[:, 1, :], in_=ssl_in[sc * 128:(sc + 1) * 128, kh * 512:(kh + 1) * 512]), writes=[Bc_])

                                def ff(E, a_=a_, c_=c_, sc=sc):
                                    r = None
                                    for ct_ in range(8):
                                        E.matmul(ps[ct_][:, :], a_[:, ct_ * 128:(ct_ + 1) * 128], c_[:, 0, :], start=(sc == 0), stop=False)
                                        r = E.matmul(ps[ct_][:, :], a_[:, 1024 + ct_ * 128:1024 + (ct_ + 1) * 128], c_[:, 1, :],
                                                     start=False, stop=(sc == 63))
                                    return r
                                S.op("pe", ff, reads=[Ba_, Bc_], writes=psB)
                            for ct_ in range(8):
                                evac(ct_, yfT[:, ct_, kh * 512:(kh + 1) * 512], ps[ct_][:, :], [psB[ct_]], [ByfT], scale=ISQ_S)
                        S.barrier()
                    if stop == "fourier":
                        dbg_out["yfT"] = dout("dbg_yfT", [128, 8 * T], BF16)
                        S.dma("sp", lambda E: E.dma_start(out=dbg_out["yfT"], in_=yfT[:].rearrange("p a t -> p (a t)")),
                              reads=[ByfT], writes=[Bdram["out"]])
                        return True
                    mT = alloc(ynaT_stack, "mT", [128, 16, T], BF16)
                    BmT = Buf("mT")
                    with ExitStack() as ph:
                        wn = [alloc(ph, f"wn{i}", [128, 8, 256], BF16) for i in range(2)]
                        wf = [alloc(ph, f"wf{i}", [128, 8, 256], BF16) for i in range(2)]
                        wga = [alloc(ph, f"wga{i}", [128, 16, 256], BF16) for i in range(2)]
                        wgf = [alloc(ph, f"wgf{i}", [128, 16, 256], BF16) for i in range(2)]
                        Bwo = [Buf(), Buf()]
                        bbg = alloc(ph, "bbg", [128, 32], F32)
                        Bbbg = Buf()
                        S.dma("sp", lambda E: E.dma_start(out=bbg[:], in_=bbg_in[l]), writes=[Bbbg])
                        ga = [alloc(ph, f"ga{i}", [128, 512], F32) for i in range(2)]
                        gf = [alloc(ph, f"gf{i}", [128, 512], F32) for i in range(2)]
                        t1 = [alloc(ph, f"t1{i}", [128, 512], F32) for i in range(2)]
                        Bga = [Buf(), Buf()]
                        Bgf = [Buf(), Buf()]
                        Bt1 = [Buf(), Buf()]
                        un = 0
                        for cb in range(8):
                            i2 = cb % 2
                            for qn, (dst, src) in enumerate(((wn[i2], wview("w_nao", l)[:, cb * 256:(cb + 1) * 256]), (wf[i2], wview("w_fo", l)[:, cb * 256:(cb + 1) * 256]),
                                             (wga[i2], wview("w_bg", l)[:, cb * 256:(cb + 1) * 256]),
                                             (wgf[i2], wview("w_bg", l)[:, 2048 + cb * 256:2048 + (cb + 1) * 256]))):
                                S.dma(("sp", "act")[qn % 2], lambda E, dst=dst, src=src: E.dma_start(out=dst[:], in_=src.rearrange("(kc p) n -> p kc n", p=128)),
                                      reads=[Bwall[("w_nao", l)], Bwall[("w_fo", l)], Bwall[("w_bg", l)]], writes=[Bwo[i2]])
                            for dcl in range(2):
                                dc = cb * 2 + dcl
                                cs_ = slice(dcl * 128, (dcl + 1) * 128)
                                for th in range(2):
                                    ts_ = slice(th * 512, (th + 1) * 512)
                                    u2 = un % 2
                                    un += 1
                                    p_na, p_f, p_ga, p_gf = nextps(), nextps(), nextps(), nextps()
                                    mm_group(p_ga, ps[p_ga][:, :], [(wga[i2][:, kc, cs_], hT[:, kc, ts_]) for kc in range(16)],
                                             reads=[Bwo[i2]] + BhT)
                                    mm_group(p_gf, ps[p_gf][:, :], [(wgf[i2][:, kc, cs_], hT[:, kc, ts_]) for kc in range(16)],
                                             reads=[Bwo[i2]] + BhT)
                                    mm_group(p_na, ps[p_na][:, :], [(wn[i2][:, kc, cs_], ynaT[:, kc, ts_]) for kc in range(8)],
                                             reads=[Bwo[i2], BynaT])
                                    mm_group(p_f, ps[p_f][:, :], [(wf[i2][:, kc, cs_], yfT[:, kc, ts_]) for kc in range(8)],
                                             reads=[Bwo[i2], ByfT])
                                    S.op("act", lambda E, u2=u2, p_ga=p_ga, dc=dc: E.activation(
                                        out=ga[u2][:], in_=ps[p_ga][:, :], func=AF.Sigmoid, bias=bbg[:, dc:dc + 1]),
                                        reads=[psB[p_ga], Bbbg], writes=[Bga[u2]])
                                    S.op("act", lambda E, u2=u2, p_gf=p_gf, dc=dc: E.activation(
                                        out=gf[u2][:], in_=ps[p_gf][:, :], func=AF.Sigmoid, bias=bbg[:, 16 + dc:17 + dc]),
                                        reads=[psB[p_gf], Bbbg], writes=[Bgf[u2]])
                                    S.op("dve", lambda E, u2=u2, p_na=p_na: E.tensor_tensor(out=t1[u2][:], in0=ps[p_na][:, :], in1=ga[u2][:],
                                                                                          op=ALU.mult), reads=[psB[p_na], Bga[u2]], writes=[Bt1[u2]])
                                    S.op("dve", lambda E, u2=u2, p_f=p_f: E.tensor_tensor(out=gf[u2][:], in0=ps[p_f][:, :], in1=gf[u2][:],
                                                                                        op=ALU.mult), reads=[psB[p_f], Bgf[u2]], writes=[Bgf[u2]])
                                    S.op("dve", lambda E, u2=u2, dc=dc, ts_=ts_: E.tensor_tensor(out=mT[:, dc, ts_], in0=t1[u2][:], in1=gf[u2][:],
                                                                                                op=ALU.add), reads=[Bt1[u2], Bgf[u2]], writes=[BmT])
                        S.barrier()
                    if stop == "merge":
                        dbg_out["mT"] = dout("dbg_mT", [128, 16 * T], BF16)
                        S.dma("sp", lambda E: E.dma_start(out=dbg_out["mT"], in_=mT[:].rearrange("p a t -> p (a t)")),
                              reads=[BmT], writes=[Bdram["out"]])
                        return True
                    with ExitStack() as ph:
                        wo = [alloc(ph, f"wo{i}", [128, 16, 512], BF16) for i in range(2)]
                        Bwo2 = [Buf(), Buf()]
                        G1b = alloc(ph, "G1b", [128, D], F32)
                        BG1 = Buf()
                        load_bcast_mod(G1b, l, 2, BG1)
                        xs = [alloc(ph, f"xs{i}", [128, 512], F32) for i in range(3)]
                        tm = [alloc(ph, f"tm{i}", [128, 512], F32) for i in range(2)]
                        Bxs = [Buf() for _ in range(3)]
                        Btm = [Buf(), Buf()]
                        n_ = 0
                        for cbk in range(4):
                            w = wo[cbk % 2]
                            S.dma("act", lambda E, w=w, cbk=cbk: E.dma_start(
                                out=w[:], in_=wview("w_out", l)[:, cbk * 512:(cbk + 1) * 512].rearrange("(kc p) n -> p kc n", p=128)),
                                reads=[Bwall[("w_out", l)]], writes=[Bwo2[cbk % 2]])
                            cs_ = slice(cbk * 512, (cbk + 1) * 512)
                            for tt in range(8):
                                x_ = xs[n_ % 3]
                                Bx_ = Bxs[n_ % 3]
                                t_ = tm[n_ % 2]
                                Bt_ = Btm[n_ % 2]
                                n_ += 1
                                S.dma("sp", lambda E, x_=x_, tt=tt, cs_=cs_: E.dma_start(out=x_[:], in_=xres[tt * 128:(tt + 1) * 128, cs_]),
                                      reads=[Bx[tt]], writes=[Bx_])
                                pi = nextps()
                                mm_group(pi, ps[pi][:, :], [(mT[:, kc, tt * 128:(tt + 1) * 128], w[:, kc, :]) for kc in range(16)],
                                         reads=[BmT, Bwo2[cbk % 2]])
                                S.op("dve", lambda E, pi=pi, t_=t_, cs_=cs_: E.tensor_tensor(out=t_[:], in0=ps[pi][:, :], in1=G1b[:, cs_], op=ALU.mult),
                                     reads=[psB[pi], BG1], writes=[Bt_])
                                S.op("dve", lambda E, x_=x_, t_=t_: E.tensor_tensor(out=x_[:], in0=x_[:], in1=t_[:], op=ALU.add),
                                     reads=[Bt_, Bx_], writes=[Bx_])
                                S.dma("sp", lambda E, x_=x_, tt=tt, cs_=cs_: E.dma_start(out=xres[tt * 128:(tt + 1) * 128, cs_], in_=x_[:]),
                                      reads=[Bx_], writes=[Bx[tt]])
                        S.barrier()
            return False

        def moe(l):
            with ExitStack() as mo:
                h2T = alloc(mo, "h2T", [128, 16, T], BF16)
                Bh2T = [Buf(f"h2T{i}") for i in range(8)]
                wts = alloc(mo, "wts", [128, 8, 2], F32)
                sli = alloc(mo, "sli", [128, 8, 2], I32)
                Bws = [Buf() for _ in range(8)]
                with ExitStack() as ph:
                    wrt = alloc(ph, "wrt", [128, 16, 36], BF16)
                    brb = alloc(ph, "brb", [128, 36], F32)
                    Bwr = Buf()
                    S.dma("pool", lambda E: E.dma_start(out=wrt[:], in_=wr_in[l].rearrange("(kc p) n -> p kc n", p=128)), writes=[Bwr])
                    load_bcast_row(brb, br_in[l:l + 1, :], Bwr)
                    selall = alloc(ph, "selall", [128, 8, 32], BF16)
                    Bsel = [Buf() for _ in range(8)]
                    R = {}
                    for nm, w_ in (("lg", 36), ("gm", 4), ("pen", 4), ("junk", 4), ("em", 32), ("mk1", 32), ("em2", 32), ("mk2", 32),
                                   ("sv", 32), ("ov", 32), ("tmp32", 32), ("sc", 16)):
                        R[nm] = [alloc(ph, f"r_{nm}{i}", [128, w_], F32) for i in range(2)]
                    Br = [Buf(), Buf()]

                    def router(tt, h_, Bh_):
                        i2 = tt % 2
                        lg, gm, pen, junk, em, mk1, em2, mk2, sv, ov, tmp32, sc = [R[k_][i2] for k_ in
                            ("lg", "gm", "pen", "junk", "em", "mk1", "em2", "mk2", "sv", "ov", "tmp32", "sc")]
                        B = Br[i2]
                        pi = nextps()
                        mm_group(pi, ps[pi][:, 0:36], [(h2T[:, kc, tt * 128:(tt + 1) * 128], wrt[:, kc, :]) for kc in range(16)],
                                 reads=[Bh2T[tt], Bwr])
                        V = lambda f, rd=(), wr=(): S.op("dve", f, reads=[B] + list(rd), writes=[B] + list(wr))
                        V(lambda E: E.tensor_tensor(out=lg[:], in0=ps[pi][:, 0:36], in1=brb[:], op=ALU.add), rd=[psB[pi], Bwr])
                        V(lambda E: E.reduce_max(out=sc[:, 0:1], in_=lg[:, 0:4], axis=AX.X))
                        V(lambda E: E.tensor_scalar(out=gm[:], in0=lg[:, 0:4], scalar1=sc[:, 0:1], scalar2=None, op0=ALU.is_equal))
                        V(lambda E: E.tensor_scalar(out=sc[:, 1:2], in0=sc[:, 0:1], scalar1=-1.0, scalar2=None, op0=ALU.mult))
                        S.op("act", lambda E: E.activation(out=junk[:], in_=lg[:, 0:4], func=AF.Exp, bias=sc[:, 1:2], accum_out=sc[:, 2:3]),
                             reads=[B], writes=[B])
                        V(lambda E: E.reciprocal(out=sc[:, 3:4], in_=sc[:, 2:3]))
                        V(lambda E: E.tensor_scalar(out=pen[:], in0=gm[:], scalar1=-1.0, scalar2=1e30, op0=ALU.add, op1=ALU.mult))
                        V(lambda E: E.tensor_tensor(out=em[:].rearrange("p (g e) -> p g e", g=4), in0=lg[:, 4:36].rearrange("p (g e) -> p g e", g=4),
                                                    in1=pen[:].unsqueeze(2).to_broadcast([128, 4, 8]), op=ALU.add))
                        V(lambda E: E.reduce_max(out=sc[:, 4:5], in_=em[:], axis=AX.X))
                        V(lambda E: E.tensor_scalar(out=mk1[:], in0=em[:], scalar1=sc[:, 4:5], scalar2=None, op0=ALU.is_equal))
                        V(lambda E: E.scalar_tensor_tensor(out=em2[:], in0=mk1[:], scalar=-1e30, in1=em[:], op0=ALU.mult, op1=ALU.add))
                        V(lambda E: E.reduce_max(out=sc[:, 6:7], in_=em2[:], axis=AX.X))
                        V(lambda E: E.tensor_scalar(out=mk2[:], in0=em2[:], scalar1=sc[:, 6:7], scalar2=None, op0=ALU.is_equal))
                        V(lambda E: E.tensor_scalar(out=sc[:, 5:6], in0=sc[:, 4:5], scalar1=-1.0, scalar2=None, op0=ALU.mult))
                        S.op("act", lambda E: E.activation(out=sc[:, 7:8], in_=sc[:, 6:7], func=AF.Exp, bias=sc[:, 5:6]), reads=[B], writes=[B])
                        V(lambda E: E.tensor_scalar(out=sc[:, 8:9], in0=sc[:, 7:8], scalar1=1.0, scalar2=None, op0=ALU.add))
                        V(lambda E: E.reciprocal(out=sc[:, 9:10], in_=sc[:, 8:9]))
                        V(lambda E: E.tensor_tensor(out=selall[:, tt, :], in0=mk1[:], in1=mk2[:], op=ALU.add), wr=[Bsel[tt]])
                        p2 = nextps()
                        mm_group(p2, ps[p2][:, 0:32], [(tri[:, 0:128], selall[:, tt, :])] + [(tri[:, 128:256], selall[:, j, :]) for j in range(tt)],
                                 reads=[Bconst] + Bsel[0:tt + 1])
                        V(lambda E: E.tensor_tensor(out=sv[:], in0=ps[p2][:, 0:32], in1=eslot[:, 0:32], op=ALU.add), rd=[psB[p2], Bconst])
                        V(lambda E: E.tensor_scalar(out=ov[:], in0=ps[p2][:, 0:32], scalar1=float(CAP), scalar2=None, op0=ALU.is_ge), rd=[psB[p2]])
                        for j, mk in enumerate((mk1, mk2)):
                            cs1, co1 = 10 + 2 * j, 11 + 2 * j
                            V(lambda E, mk=mk: E.tensor_tensor(out=tmp32[:], in0=mk[:], in1=sv[:], op=ALU.mult))
                            V(lambda E, cs1=cs1: E.reduce_sum(out=sc[:, cs1:cs1 + 1], in_=tmp32[:], axis=AX.X))
                            V(lambda E, mk=mk: E.tensor_tensor(out=tmp32[:], in0=mk[:], in1=ov[:], op=ALU.mult))
                            V(lambda E, co1=co1: E.reduce_sum(out=sc[:, co1:co1 + 1], in_=tmp32[:], axis=AX.X))
                            V(lambda E, cs1=cs1: E.tensor_tensor(out=sc[:, 14:15], in0=eslot[:, 32:33], in1=sc[:, cs1:cs1 + 1], op=ALU.subtract), rd=[Bconst])
                            V(lambda E, cs1=cs1, co1=co1: E.scalar_tensor_tensor(out=sc[:, cs1:cs1 + 1], in0=sc[:, 14:15], scalar=sc[:, co1:co1 + 1],
                                                                               in1=sc[:, cs1:cs1 + 1], op0=ALU.mult, op1=ALU.add))
                            V(lambda E, cs1=cs1, j=j: E.tensor_copy(out=sli[:, tt, j:j + 1], in_=sc[:, cs1:cs1 + 1]), wr=[Bws[tt]])
                            V(lambda E, co1=co1: E.tensor_scalar(out=sc[:, 14:15], in0=sc[:, co1:co1 + 1], scalar1=-1.0, scalar2=1.0, op0=ALU.mult, op1=ALU.add))
                            V(lambda E: E.tensor_tensor(out=sc[:, 14:15], in0=sc[:, 14:15], in1=sc[:, 3:4], op=ALU.mult))
                            V(lambda E: E.tensor_tensor(out=sc[:, 14:15], in0=sc[:, 14:15], in1=sc[:, 9:10], op=ALU.mult))
                            if j == 0:
                                V(lambda E: E.tensor_copy(out=wts[:, tt, 0:1], in_=sc[:, 14:15]), wr=[Bws[tt]])
                            else:
                                V(lambda E: E.tensor_tensor(out=wts[:, tt, 1:2], in0=sc[:, 14:15], in1=sc[:, 7:8], op=ALU.mult), wr=[Bws[tt]])
                            if stop != "route":
                                S.barrier(engs=("pool",))
                                S.dma("pool", lambda E, j=j, h_=h_: E.indirect_dma_start(
                                    out=xg, out_offset=bass.IndirectOffsetOnAxis(sli[:, tt, j:j + 1], 0), in_=h_[:], in_offset=None),
                                    reads=[Bh_, Bws[tt]], writes=[Bdram["xg"]])

                    phase_norm(ph, l, n2g_in[l:l + 1, :], 3, 4, xres, Bx, h2T, Bh2T, post_hook=router)
                    S.barrier()
                if stop in ("route", "route2"):
                    dbg_out["wts"] = dout("dbg_wts", [128, 16], F32)
                    dbg_out["sli"] = dout("dbg_sli", [128, 16], I32)
                    S.dma("sp", lambda E: E.dma_start(out=dbg_out["wts"], in_=wts[:].rearrange("p a b -> p (a b)")), reads=Bws, writes=[Bdram["out"]])
                    S.dma("sp", lambda E: E.dma_start(out=dbg_out["sli"], in_=sli[:].rearrange("p a b -> p (a b)")), reads=Bws, writes=[Bdram["out"]])
                    return True
                dist_flush()
                if l + 1 < nl:
                    dist_enqueue([sg for sg in segs if sg[1] == l + 1 and sg[0] in MIXN])
                    dist_enqueue([sg for sg in segs if sg[1] == l + 1 and sg[0] not in MIXN])
                NST = CAP // 128
                with ExitStack() as ph:
                    wgt = [alloc(ph, f"wgt{i}", [128, 16, DE], BF16) for i in range(2)]
                    wut = [alloc(ph, f"wut{i}", [128, 16, DE], BF16) for i in range(2)]
                    wdt = [alloc(ph, f"wdt{i}", [128, 4, D], BF16) for i in range(2)]
                    Bwg, Bwu, Bwd = [Buf(), Buf()], [Buf(), Buf()], [Buf(), Buf()]
                    xgt = [alloc(ph, f"xgt{i}", [128, D], BF16) for i in range(2)]
                    Bxg = [Buf(), Buf()]
                    xT = [alloc(ph, f"xT{i}", [128, 16, CAP], BF16) for i in range(2)]
                    BxT = [Buf(), Buf()]
                    sg = [alloc(ph, f"sg{i}", [128, CAP], F32) for i in range(2)]
                    Bsg = [Buf(), Buf()]
                    aT = [alloc(ph, f"aT{i}", [128, 4, CAP], BF16) for i in range(2)]
                    BaT = [Buf(), Buf()]
                    yev = [alloc(ph, f"yev{i}", [128, D], BF16) for i in range(2)]
                    Byev = [Buf(), Buf()]
                    nx, ny, ns, ec = 0, 0, 0, 0
                    for e in range(NE):
                        i2 = e % 2
                        S.dma("act", lambda E, e=e, i2=i2: E.dma_start(out=wgt[i2][:], in_=wview("w_gate", l, e).rearrange("(kc p) n -> p kc n", p=128)),
                              reads=[Bwall[("w_gate", l)]], writes=[Bwg[i2]])
                        S.dma("act", lambda E, e=e, i2=i2: E.dma_start(out=wut[i2][:], in_=wview("w_up", l, e).rearrange("(kc p) n -> p kc n", p=128)),
                              reads=[Bwall[("w_up", l)]], writes=[Bwu[i2]])
                        S.dma("act", lambda E, e=e, i2=i2: E.dma_start(out=wdt[i2][:], in_=wview("w_down", l, e).rearrange("(kc p) n -> p kc n", p=128)),
                              reads=[Bwall[("w_down", l)]], writes=[Bwd[i2]])
                        for st in range(NST):
                            g_ = xgt[nx % 2]
                            Bg_ = Bxg[nx % 2]
                            nx += 1
                            r0 = e * CAP + st * 128
                            S.dma("sp", lambda E, g_=g_, r0=r0: E.dma_start(out=g_[:], in_=xg[r0:r0 + 128, :]), reads=[Bdram["xg"]], writes=[Bg_])
                            for half in range(2):
                                pi = nextps()
                                pbf = ps[pi][:].bitcast(BF16)

                                def trf(E, g_=g_, pbf=pbf, half=half):
                                    r = None
                                    for j in range(8):
                                        kc = half * 8 + j
                                        r = E.transpose(out=pbf[:, j * 128:(j + 1) * 128], in_=g_[:, kc * 128:(kc + 1) * 128], identity=ident[:])
                                    return r
                                S.op("pe", trf, reads=[Bg_, Bconst], writes=[psB[pi]])
                                evac(ec, xT[i2][:, half * 8:(half + 1) * 8, st * 128:(st + 1) * 128], pbf.rearrange("p (k t) -> p k t", k=8),
                                     [psB[pi]], [BxT[i2]])
                                ec += 1
                        dist_pump(13)
                        for fc in range(4):
                            pg, pu = nextps(), nextps()
                            mm_group(pg, ps[pg][:, 0:CAP], [(wgt[i2][:, kc, fc * 128:(fc + 1) * 128], xT[i2][:, kc, :]) for kc in range(16)],
                                     reads=[Bwg[i2], BxT[i2]])
                            mm_group(pu, ps[pu][:, 0:CAP], [(wut[i2][:, kc, fc * 128:(fc + 1) * 128], xT[i2][:, kc, :]) for kc in range(16)],
                                     reads=[Bwu[i2], BxT[i2]])
                            s_ = sg[ns % 2]
                            Bs_ = Bsg[ns % 2]
                            ns += 1
                            S.op("act", lambda E, s_=s_, pg=pg: E.activation(out=s_[:], in_=ps[pg][:, 0:CAP], func=AF.Silu), reads=[psB[pg]], writes=[Bs_])
                            S.op("dve", lambda E, s_=s_, pu=pu, fc=fc, i2=i2: E.tensor_tensor(out=aT[i2][:, fc, :], in0=ps[pu][:, 0:CAP], in1=s_[:], op=ALU.mult),
                                 reads=[psB[pu], Bs_], writes=[BaT[i2]])
                        for st in range(NST):
                            y_ = yev[ny % 2]
                            By_ = Byev[ny % 2]
                            ny += 1
                            for nb in range(4):
                                pi = nextps()
                                mm_group(pi, ps[pi][:, :], [(aT[i2][:, fc, st * 128:(st + 1) * 128], wdt[i2][:, fc, nb * 512:(nb + 1) * 512]) for fc in range(4)],
                                         reads=[BaT[i2], Bwd[i2]])
                                evac(ec, y_[:, nb * 512:(nb + 1) * 512], ps[pi][:, :], [psB[pi]], [By_])
                                ec += 1
                            r0 = e * CAP + st * 128
                            S.dma("sp", lambda E, y_=y_, r0=r0: E.dma_start(out=yg[r0:r0 + 128, :], in_=y_[:]), reads=[By_], writes=[Bdram["yg"]])
                    S.barrier()
                dist_flush()
                with ExitStack() as ph:
                    G2b = alloc(ph, "G2b", [128, D], F32)
                    BG2 = Buf()
                    load_bcast_mod(G2b, l, 5, BG2)
                    y1 = [alloc(ph, f"y1{i}", [128, D], BF16) for i in range(2)]
                    y2 = [alloc(ph, f"y2{i}", [128, D], BF16) for i in range(2)]
                    xc = [alloc(ph, f"xc{i}", [128, D], F32) for i in range(2)]
                    acc = [alloc(ph, f"acc{i}", [128, D], F32) for i in range(2)]
                    By1, By2, Bxc, Bacc = [Buf(), Buf()], [Buf(), Buf()], [Buf(), Buf()], [Buf(), Buf()]
                    for tt in range(8):
                        i2 = tt % 2
                        S.barrier(engs=("pool",))
                        S.dma("pool", lambda E, tt=tt, i2=i2: E.indirect_dma_start(out=y1[i2][:], out_offset=None, in_=yg,
                              in_offset=bass.IndirectOffsetOnAxis(sli[:, tt, 0:1], 0)), reads=[Bdram["yg"], Bws[tt]], writes=[By1[i2]])
                        S.dma("pool", lambda E, tt=tt, i2=i2: E.indirect_dma_start(out=y2[i2][:], out_offset=None, in_=yg,
                              in_offset=bass.IndirectOffsetOnAxis(sli[:, tt, 1:2], 0)), reads=[Bdram["yg"], Bws[tt]], writes=[By2[i2]])
                        S.dma("sp", lambda E, tt=tt, i2=i2: E.dma_start(out=xc[i2][:], in_=xres[tt * 128:(tt + 1) * 128, :]), reads=[Bx[tt]], writes=[Bxc[i2]])
                        S.op("dve", lambda E, tt=tt, i2=i2: E.tensor_scalar(out=acc[i2][:], in0=y1[i2][:], scalar1=wts[:, tt, 0:1], scalar2=None, op0=ALU.mult),
                             reads=[By1[i2], Bws[tt]], writes=[Bacc[i2]])
                        S.op("dve", lambda E, tt=tt, i2=i2: E.scalar_tensor_tensor(out=acc[i2][:], in0=y2[i2][:], scalar=wts[:, tt, 1:2], in1=acc[i2][:],
                                                                                 op0=ALU.mult, op1=ALU.add), reads=[By2[i2], Bws[tt], Bacc[i2]], writes=[Bacc[i2]])
                        S.op("dve", lambda E, i2=i2: E.tensor_tensor(out=acc[i2][:], in0=acc[i2][:], in1=G2b[:], op=ALU.mult), reads=[BG2, Bacc[i2]], writes=[Bacc[i2]])
                        S.op("dve", lambda E, i2=i2: E.tensor_tensor(out=xc[i2][:], in0=xc[i2][:], in1=acc[i2][:], op=ALU.add), reads=[Bacc[i2], Bxc[i2]], writes=[Bxc[i2]])
                        S.dma("sp", lambda E, tt=tt, i2=i2: E.dma_start(out=xres[tt * 128:(tt + 1) * 128, :], in_=xc[i2][:]), reads=[Bxc[i2]], writes=[Bx[tt]])
                    S.barrier()
            return False

        with ExitStack() as ph:
            zt2 = alloc(ph, "zt2", [128, D], BF16)
            Bz2 = Buf()
            S.op("pool", lambda E: E.memset(zt2[:], 0.0), writes=[Bz2])
            S.dma("sp", lambda E: E.dma_start(out=yg[NE * CAP:NE * CAP + 128, :], in_=zt2[:]), reads=[Bz2], writes=[Bdram["yg"]])
            S.barrier()

        stopped = False
        for l in range(nl):
            if mixer(l):
                stopped = True
                break
            if stop == "mix" and l == nl - 1:
                break
            if moe(l):
                stopped = True
                break
        if not stopped:
            if stop in ("mix", "moe"):
                with ExitStack() as ph:
                    t2 = [alloc(ph, f"fo{i}", [128, D], F32) for i in range(2)]
                    Bt2 = [Buf(), Buf()]
                    for tt in range(8):
                        S.dma("sp", lambda E, tt=tt: E.dma_start(out=t2[tt % 2][:], in_=xres[tt * 128:(tt + 1) * 128, :]), reads=[Bx[tt]], writes=[Bt2[tt % 2]])
                        S.dma("sp", lambda E, tt=tt: E.dma_start(out=out[tt * 128:(tt + 1) * 128, :], in_=t2[tt % 2][:]), reads=[Bt2[tt % 2]], writes=[Bdram["out"]])
            else:
                with ExitStack() as ph:
                    phase_norm(ph, 0, fing_in[0:1, :], 0, 0, xres, Bx, None, None, final=True)
        for e_ in ENGS:
            for t_ in S.final_tokens():
                S._wait(e_, t_)
        S.emit()
    return nc, dbg_out


_CONST = {}


def _consts():
    if _CONST:
        return _CONST
    p = np.arange(128)
    cd = np.zeros((128, 2, 512), np.float64)
    for mc in range(2):
        m = (mc * 128 + p)[:, None]
        j = np.arange(256)[None, :]
        ang = 2 * np.pi * ((m * j) % 256) / 256.0
        cd[:, mc, 0:256] = np.cos(ang)
        cd[:, mc, 256:512] = np.sin(ang)
    _CONST["cdft"] = cd.astype(np.float32).astype(BF)
    _CONST["ident"] = np.eye(128, dtype=np.float32).astype(BF)
    tri = np.zeros((128, 256), np.float32)
    tri[:, 0:128] = (p[:, None] < p[None, :]).astype(np.float32)
    tri[:, 128:256] = 1.0
    _CONST["tri"] = tri.astype(BF)
    s = np.arange(8192, dtype=np.int64)[:, None]
    csl, ssl = [], []
    for c in range(NCORE):
        k = (c * 1024 + np.arange(1024, dtype=np.int64))[None, :]
        ang = 2 * np.pi * ((s * k) % 8192).astype(np.float64) / 8192.0
        csl.append(np.cos(ang).astype(np.float32).astype(BF))
        ssl.append((-np.sin(ang)).astype(np.float32).astype(BF))
    _CONST["csl"] = csl
    _CONST["ssl"] = ssl
    es = np.zeros((128, NE + 1), np.float32)
    es[:, :NE] = (np.arange(NE) * CAP)[None, :]
    es[:, NE] = NE * CAP + p
    _CONST["eslot"] = es
    return _CONST


def _na_bias_tables(rpb, c):
    L, H = rpb.shape[0], rpb.shape[1]
    out = np.full((L, H, 3, 128, 6, 4, 64), -1e30, np.float32)
    kcol = np.arange(64)
    qc = np.arange(64)
    cs = np.clip(qc - 8, 0, 48)
    cmask = (kcol[:, None] >= cs[None, :]) & (kcol[:, None] < cs[None, :] + 16)
    dc = np.clip(kcol[:, None] - qc[None, :], -15, 15) + 15
    for ci, b in enumerate((0, 1, 3)):
        for i in range(4):
            r = 16 * c + 4 * b + i
            rs = min(max(r - 4, 0), 120)
            for j in range(12):
                gk = 16 * c - 4 + 4 * b + j
                if gk < rs or gk >= rs + 8 or gk < 0 or gk > 127:
                    continue
                dr = gk - r + 7
                vals = rpb[:, :, dr][:, :, dc]
                vals = np.where(cmask[None, None], vals, np.float32(-1e30))
                kc, jj = j // 2, j % 2
                out[:, :, ci, jj * 64:(jj + 1) * 64, kc, i, :] = vals
    return out.reshape(L, H, 3, 128, 1536)


def _make_in_maps(inp, nl=NL, stop=None):
    C = _consts()
    f32 = np.float32
    x = np.asarray(inp["x"], f32)[0]
    cvec = np.asarray(inp["c"], f32)[0]
    ada_w = np.asarray(inp["ada_w"], f32)
    ada_b = np.asarray(inp["ada_b"], f32)
    shared = {
        "cT": np.ascontiguousarray(cvec.reshape(16, 128).T),
        "n1g": np.asarray(inp["norm1_g"], f32),
        "n2g": np.asarray(inp["norm2_g"], f32),
        "fing": np.asarray(inp["final_g"], f32).reshape(1, D),
        "b_bgT": np.ascontiguousarray(np.asarray(inp["branch_gate_b"], f32).reshape(NL, 32, 128).transpose(0, 2, 1)),
        "w_r": np.ascontiguousarray(np.concatenate([np.asarray(inp["router_group_w"], f32), np.asarray(inp["router_expert_w"], f32)], axis=2)),
        "b_r": np.ascontiguousarray(np.concatenate([np.asarray(inp["router_group_b"], f32), np.asarray(inp["router_expert_b"], f32)], axis=1)),
        "cdft": C["cdft"], "ident": C["ident"], "tri": C["tri"], "eslot": C["eslot"],
    }
    srcs = {"w_in": "mix_in_w", "w_nao": "na_out_w", "w_fo": "fourier_out_w", "w_bg": "branch_gate_w", "w_out": "mix_out_w",
            "w_gate": "expert_w_gate", "w_up": "expert_w_up", "w_down": "expert_w_down"}
    segs = seg_plan(nl, stop)
    flat = [(np.asarray(inp[srcs[n]][l], f32).reshape(-1, 2048), r) for n, l, r in segs]
    for a, r in flat:
        assert a.shape[0] == r
    rpb = np.asarray(inp["na_rpb"], f32)
    variants = np.stack([_na_bias_tables(rpb, 1)[:, :, 1], _na_bias_tables(rpb, 0)[:, :, 0],
                         _na_bias_tables(rpb, NCORE - 1)[:, :, 2]], axis=0)
    vflat = variants.reshape(-1, 2048)
    assert vflat.shape[0] == BROWS
    bown = BROWS // NCORE
    p = np.arange(128)
    maps = []
    for c in range(NCORE):
        m = dict(shared)
        m["x"] = np.ascontiguousarray(x[c * T:(c + 1) * T])
        cols = (np.arange(6)[:, None] * 2048 + c * 256 + np.arange(256)[None, :]).reshape(-1)
        m["adaw"] = np.ascontiguousarray(ada_w[:, :, cols])
        m["adab"] = np.ascontiguousarray(ada_b[:, cols]).reshape(1, NL * 1536)
        sel = np.zeros((1, 128), f32)
        for l in range(NL):
            sel[0, l * 32 + c * 4 + l] = 1.0
        m["selrow"] = sel
        parts, cols_ = [], []
        off = 0
        for a, r in flat:
            own = r // NCORE
            parts.append(a[c * own:(c + 1) * own])
            for j in range(own // 128):
                cols_.append(c * own + j * 128 + p)
            off += r
        if parts:
            m["wshard"] = np.ascontiguousarray(np.concatenate(parts, axis=0))
            m["witab"] = np.ascontiguousarray(np.stack(cols_, axis=1).astype(np.int32))
        else:
            m["wshard"] = np.zeros((128, 2048), f32)
            m["witab"] = np.zeros((128, 1), np.int32)
        m["bshard"] = np.ascontiguousarray(vflat[c * bown:(c + 1) * bown])
        bi = np.zeros((128, NL * 48 + bown // 128), np.int32)
        for l in range(NL):
            for h in range(16):
                for k3 in range(3):
                    v = 0
                    if k3 == 0 and c == 0:
                        v = 1
                    if k3 == 2 and c == NCORE - 1:
                        v = 2
                    bi[:, (l * 16 + h) * 3 + k3] = ((v * NL + l) * 16 + h) * 128 + p
        for t in range(bown // 128):
            bi[:, NL * 48 + t] = c * bown + t * 128 + p
        m["bidx"] = bi
        m["csl"] = C["csl"][c]
        m["ssl"] = C["ssl"][c]
        ca, cb = max(c - 1, 0), min(c + 1, NCORE - 1)
        it = np.zeros((128, 24), np.int32)
        for tt in range(8):
            it[:, tt] = c * 1024 + tt * 128 + p
        it[:, 8] = c * 128 + p
        it[:, 9] = c * 256 + p
        it[:, 10] = c * 256 + 128 + p
        it[:, 11] = c * 256 + p
        it[:, 12] = c * 256 + 128 + p
        it[:, 13] = ca * 128 + p
        it[:, 14] = cb * 128 + p
        it[:, 15] = ca * 256 + p
        it[:, 16] = ca * 256 + 128 + p
        it[:, 17] = cb * 256 + p
        it[:, 18] = cb * 256 + 128 + p
        m["itab"] = it
        fl = np.zeros((128, 2), f32)
        fl[:, 0] = 1.0 if c > 0 else 0.0
        fl[:, 1] = 1.0 if c < NCORE - 1 else 0.0
        m["flags"] = fl
        maps.append(m)
    return maps


_PROG = {}


def kernel(**inputs):
    if "full" not in _PROG:
        _PROG["full"] = build()[0]
    nc = _PROG["full"]
    maps = _make_in_maps(inputs)
    res = run_bass_kernel_spmd(nc, maps, core_ids=list(range(NCORE)))
    outp = np.concatenate([np.asarray(res.results[c]["out"], np.float32) for c in range(NCORE)], axis=0)
    return outp.reshape(1, NCORE * T, D)
```
